# Optimizing a Trainium2 kernel written in Bass

```python
import jax, jax.numpy as jnp
from jax import lax
import numpy as np

D_MODEL = 4096
BATCH = 4
SEQ = 4096
DEPTH = 4

GLA_HEADS = 8
GLA_DK = 128
GLA_DV = 256
GLA_QK_WIDTH = GLA_HEADS * GLA_DK
GLA_V_WIDTH = GLA_HEADS * GLA_DV
GLA_DECAY_RANK = 16
GLA_GATE_NORMALIZER = 16.0
GLA_LOG_DECAY_MIN = -1.0
GLA_CHUNK = 64
SWA_Q_HEADS = 32
SWA_KV_HEADS = 8
SWA_HEAD_DIM = 64
SWA_WINDOW = 128
SWA_Q_WIDTH = SWA_Q_HEADS * SWA_HEAD_DIM
SWA_KV_WIDTH = SWA_KV_HEADS * SWA_HEAD_DIM

IN_SPLITS = (GLA_QK_WIDTH, GLA_QK_WIDTH, GLA_V_WIDTH, GLA_V_WIDTH, GLA_DECAY_RANK,
             SWA_Q_WIDTH, SWA_KV_WIDTH, SWA_KV_WIDTH, SWA_Q_WIDTH, D_MODEL, D_MODEL)
IN_WIDTH = sum(IN_SPLITS)
RMS_EPS = 1e-6

kernel_name = "gla_swa_sink_gated_hybrid"


def rms_norm(x, g):
    xf = x.astype(jnp.float32)
    y = xf * lax.rsqrt(jnp.mean(xf * xf, axis=-1, keepdims=True) + RMS_EPS)
    return (y * g.astype(jnp.float32)).astype(x.dtype)


def split_columns(z, sizes):
    out, off = [], 0
    for s in sizes:
        out.append(z[..., off:off + s])
        off += s
    return out


def gla_mix(q, k, v, log_a):
    B_, S_, H, DK = q.shape
    DV = v.shape[-1]
    C = GLA_CHUNK
    N = S_ // C

    def chunk(t):
        return t.astype(jnp.float32).reshape(B_, N, C, H, t.shape[-1]).transpose(0, 3, 1, 2, 4)

    qf = chunk(q) * (DK ** -0.5)
    kf, vf, la = chunk(k), chunk(v), chunk(log_a)
    b = jnp.cumsum(la, axis=3)
    b_ref = b[:, :, :, C // 2:C // 2 + 1]
    b_last = b[:, :, :, C - 1:C]
    A = jnp.einsum('bhnid,bhnjd->bhnij', qf * jnp.exp(b - b_ref), kf * jnp.exp(b_ref - b))
    causal = jnp.tril(jnp.ones((C, C), dtype=bool))
    A = jnp.where(causal, A, 0.0)
    o_intra = jnp.einsum('bhnij,bhnjv->bhniv', A, vf)
    q_inter = qf * jnp.exp(b)
    k_state = kf * jnp.exp(b_last - b)
    decay = jnp.exp(b_last[:, :, :, 0, :])

    def step(state, xs):
        qn, kn, vn, dn = xs
        o = jnp.einsum('bhcd,bhdv->bhcv', qn, state)
        state = dn[..., None] * state + jnp.einsum('bhcd,bhcv->bhdv', kn, vn)
        return state, o

    xs = (jnp.moveaxis(q_inter, 2, 0), jnp.moveaxis(k_state, 2, 0),
          jnp.moveaxis(vf, 2, 0), jnp.moveaxis(decay, 2, 0))
    s0 = jnp.zeros((B_, H, DK, DV), jnp.float32)
    _, o_inter = lax.scan(step, s0, xs)
    o = o_intra + jnp.moveaxis(o_inter, 0, 2)
    return o.transpose(0, 2, 3, 1, 4).reshape(B_, S_, H, DV)


def swa_sink_mix(q, k, v, sinks):
    B_, S_, HQ, HD = q.shape
    G = SWA_KV_HEADS
    R = HQ // G
    W = SWA_WINDOW
    N = S_ // W
    qb = q.astype(jnp.float32).reshape(B_, N, W, G, R, HD)

    def band(t):
        tp = jnp.pad(t.astype(jnp.float32), ((0, 0), (W, 0), (0, 0), (0, 0))).reshape(B_, N + 1, W, G, HD)
        return jnp.concatenate([tp[:, :-1], tp[:, 1:]], axis=2)

    kb, vb = band(k), band(v)
    s = jnp.einsum('bnqgrd,bnkgd->bngrqk', qb, kb) * (HD ** -0.5)
    qi = jnp.arange(W)[:, None]
    kj = jnp.arange(2 * W)[None, :]
    local = (kj > qi) & (kj <= qi + W)
    key_pos = jnp.arange(N)[:, None, None] * W + kj[None] - W
    valid = local[None] & (key_pos >= 0)
    s = jnp.where(valid[None, :, None, None], s, jnp.finfo(jnp.float32).min)
    sink = sinks.astype(jnp.float32).reshape(1, 1, G, R, 1, 1)
    m = jnp.maximum(jnp.max(s, axis=-1, keepdims=True), sink)
    p = jnp.exp(s - m)
    probs = p / (jnp.sum(p, axis=-1, keepdims=True) + jnp.exp(sink - m))
    o = jnp.einsum('bngrqk,bnkgd->bnqgrd', probs, vb)
    return o.reshape(B_, S_, HQ * HD)


def setup_inputs(seed: int = 0) -> dict:
    key = jax.random.key(seed)
    ks = jax.random.split(key, 13)
    f32 = jnp.float32
    x = jax.random.normal(ks[0], (BATCH, SEQ, D_MODEL), f32)
    norm_gains = 1.0 + 0.02 * jax.random.normal(ks[1], (DEPTH, D_MODEL), f32)
    w_in = jax.random.normal(ks[2], (DEPTH, D_MODEL, IN_WIDTH), f32) * D_MODEL ** -0.5
    b_gates = 0.1 * jax.random.normal(ks[3], (DEPTH, 2, D_MODEL), f32)
    w_decay_up = jax.random.normal(ks[4], (DEPTH, GLA_DECAY_RANK, GLA_QK_WIDTH), f32) * GLA_DECAY_RANK ** -0.5
    b_decay = 0.1 * jax.random.normal(ks[5], (DEPTH, GLA_QK_WIDTH), f32)
    gla_norm_gains = 1.0 + 0.02 * jax.random.normal(ks[6], (DEPTH, GLA_DV), f32)
    sinks = 0.5 * jax.random.normal(ks[7], (DEPTH, SWA_Q_HEADS), f32)
    w_gla_out = jax.random.normal(ks[8], (DEPTH, GLA_V_WIDTH, D_MODEL), f32) * GLA_V_WIDTH ** -0.5
    w_swa_out = jax.random.normal(ks[9], (DEPTH, SWA_Q_WIDTH, D_MODEL), f32) * SWA_Q_WIDTH ** -0.5
    w_out = jax.random.normal(ks[10], (DEPTH, D_MODEL, D_MODEL), f32) * D_MODEL ** -0.5
    final_norm_gain = 1.0 + 0.02 * jax.random.normal(ks[11], (D_MODEL,), f32)
    return {"x": x, "norm_gains": norm_gains, "w_in": w_in, "b_gates": b_gates,
            "w_decay_up": w_decay_up, "b_decay": b_decay, "gla_norm_gains": gla_norm_gains,
            "sinks": sinks, "w_gla_out": w_gla_out, "w_swa_out": w_swa_out,
            "w_out": w_out, "final_norm_gain": final_norm_gain}


def reference(x, norm_gains, w_in, b_gates, w_decay_up, b_decay, gla_norm_gains,
              sinks, w_gla_out, w_swa_out, w_out, final_norm_gain):
    B_, S_, _ = x.shape
    for l in range(DEPTH):
        h = rms_norm(x, norm_gains[l])
        z = h @ w_in[l]
        (gq, gk, gv, gg, gdec, sq, sk, sv, sg, ga, gb) = split_columns(z, IN_SPLITS)
        logit = (gdec @ w_decay_up[l] + b_decay[l]).astype(jnp.float32)
        log_a = jnp.maximum(jax.nn.log_sigmoid(logit) / GLA_GATE_NORMALIZER, GLA_LOG_DECAY_MIN)
        o_a = gla_mix(gq.reshape(B_, S_, GLA_HEADS, GLA_DK),
                      gk.reshape(B_, S_, GLA_HEADS, GLA_DK),
                      gv.reshape(B_, S_, GLA_HEADS, GLA_DV),
                      log_a.reshape(B_, S_, GLA_HEADS, GLA_DK))
        o_a = rms_norm(o_a, gla_norm_gains[l]).reshape(B_, S_, GLA_V_WIDTH).astype(x.dtype)
        y_a = (o_a * jax.nn.silu(gg)) @ w_gla_out[l]
        o_b = swa_sink_mix(sq.reshape(B_, S_, SWA_Q_HEADS, SWA_HEAD_DIM),
                           sk.reshape(B_, S_, SWA_KV_HEADS, SWA_HEAD_DIM),
                           sv.reshape(B_, S_, SWA_KV_HEADS, SWA_HEAD_DIM),
                           sinks[l]).astype(x.dtype)
        y_b = (o_b * jax.nn.silu(sg)) @ w_swa_out[l]
        merged = jax.nn.sigmoid(ga + b_gates[l, 0]) * y_a + jax.nn.sigmoid(gb + b_gates[l, 1]) * y_b
        x = x + merged @ w_out[l]
    return rms_norm(x, final_norm_gain)
```

```python
import numpy as np
import concourse.bass as bass
import concourse.mybir as mybir
from concourse.bass_utils import run_bass_kernel_spmd

F32 = mybir.dt.float32
BF16 = mybir.dt.bfloat16
AF = mybir.ActivationFunctionType
ALU = mybir.AluOpType

D = 4096
KB = 32
INW = 19472
T = 512
NSLOT = 4
EPS = 1e-6
O_GQ, O_GK, O_GV, O_GG, O_DEC = 0, 1024, 2048, 4096, 6144
O_SQ, O_SK, O_SV, O_SG, O_GA, O_GB = 6160, 8208, 8720, 9232, 11280, 15376


import types


def freeze(fn):
    if fn is None or fn.__closure__ is None:
        return fn
    cells = []
    for c in fn.__closure__:
        try:
            cells.append(types.CellType(c.cell_contents))
        except ValueError:
            cells.append(c)
    return types.FunctionType(fn.__code__, fn.__globals__, fn.__name__, fn.__defaults__, tuple(cells))


class Sched:
    def __init__(self):
        self.ops = []
        self.last_w = {}
        self.readers = {}

    def op(self, eng, fn, reads=(), writes=(), dma=None, nodep=False):
        i = len(self.ops)
        deps = set()
        if not nodep:
            for b in list(reads) + list(writes):
                if b in self.last_w:
                    deps.add(self.last_w[b])
            for b in writes:
                for r in self.readers.get(b, ()):
                    deps.add(r)
        deps.discard(i)
        self.ops.append(dict(eng=eng, fn=freeze(fn), deps=deps, dma=dma, mark=False))
        for b in writes:
            self.last_w[b] = i
            self.readers[b] = []
        for b in reads:
            self.readers.setdefault(b, []).append(i)
        return i

    def emit(self, nc, block, sems, dma_sems):
        ops = self.ops
        for o in ops:
            for d in o["deps"]:
                ops[d]["mark"] = True
        cnt = {}
        for o in ops:
            if o["dma"] is not None:
                k = ("dma", o["dma"])
                cnt[k] = cnt.get(k, 0) + 16
                o["sem"] = dma_sems[o["dma"]]
                o["val"] = cnt[k]
            elif o["mark"]:
                k = o["eng"]
                if o["eng"] == "pe":
                    pass
                cnt[k] = cnt.get(k, 0) + 1
                o["sem"] = sems[o["eng"]]
                o["val"] = cnt[k]
        per_eng = {}
        for i, o in enumerate(ops):
            per_eng.setdefault(o["eng"], []).append(i)

        def run(engname, eng):
            waited = {}
            for i in per_eng.get(engname, []):
                o = ops[i]
                need = {}
                for d in o["deps"]:
                    p = ops[d]
                    if p["dma"] is None and p["eng"] == engname and engname == "pe":
                        continue
                    key = id(p["sem"])
                    if need.get(key, (None, 0))[1] < p["val"]:
                        need[key] = (p["sem"], p["val"])
                for key, (sem, val) in need.items():
                    if waited.get(key, 0) < val:
                        eng.wait_ge(sem, val)
                        waited[key] = val
                if o["fn"] is None:
                    continue
                ins = o["fn"](eng)
                if o["dma"] is not None:
                    ins.then_inc(o["sem"], 16)
                elif o["mark"]:
                    ins.then_inc(o["sem"], 1)

        @block.sync
        def _(e):
            run("sp", e)

        @block.gpsimd
        def _(e):
            run("pool", e)

        @block.tensor
        def _(e):
            run("pe", e)

        @block.scalar
        def _(e):
            run("act", e)

        @block.vector
        def _(e):
            run("dve", e)


def weight_groups():
    g = [("D", "w_in", [(O_DEC, 16)], 32)]
    for h in range(8):
        g.append((f"Q{h}", "w_in", [(O_GQ + 128 * h, 128)], 32))
        g.append((f"K{h}", "w_in", [(O_GK + 128 * h, 128)], 32))
        for vb in range(2):
            g.append((f"V{h}_{vb}", "w_in", [(O_GV + 256 * h + 128 * vb, 128)], 32))
        for vb in range(2):
            g.append((f"G{h}_{vb}", "w_in", [(O_GG + 256 * h + 128 * vb, 128)], 32))
    for cb in range(4):
        g.append((f"VS{cb}", "w_in", [(O_SV + 128 * cb, 128)], 32))
    for s in range(8):
        g.append((f"KK{s}", "w_in", [(O_SK + 64 * s, 64), (O_SK + 64 * s, 64)], 32))
        for pr in range(2):
            g.append((f"SQ{s}_{pr}", "w_in", [(O_SQ + 256 * s + 128 * pr, 128)], 32))
        for pr in range(2):
            g.append((f"SG{s}_{pr}", "w_in", [(O_SG + 256 * s + 128 * pr, 128)], 32))
    for n in range(32):
        g.append((f"GA{n}", "w_in", [(O_GA + 128 * n, 128)], 32))
        g.append((f"GB{n}", "w_in", [(O_GB + 128 * n, 128)], 32))
        g.append((f"WA{n}", "w_gla_out", [(128 * n, 128)], 16))
        g.append((f"WB{n}", "w_swa_out", [(128 * n, 128)], 16))
    for m in range(32):
        g.append((f"WO{m}", "w_out", [(128 * m, 128)], 32))
    return g


import os
DBG = int(os.environ.get('KDBG', '9'))
SUB = int(os.environ.get('KSUB', '9'))


def build_nc(NL, NT):
    S = NT * T
    nc = bass.Bass("TRN2", target_bir_lowering=False)
    dt_in = lambda name, shape, dt=F32: nc.dram_tensor(name, shape, dt, kind="ExternalInput").ap()
    xin = dt_in("xT", [KB, 128, S])
    w_in = dt_in("w_in", [NL, D, INW])
    w_gla_out = dt_in("w_gla_out", [NL, 2048, D])
    w_swa_out = dt_in("w_swa_out", [NL, 2048, D])
    w_out = dt_in("w_out", [NL, D, D])
    wsrc = {"w_in": w_in, "w_gla_out": w_gla_out, "w_swa_out": w_swa_out, "w_out": w_out}
    gains_d = dt_in("gains", [128, NL + 1, KB])
    bg_d = dt_in("bgates", [128, NL, 2, KB])
    waug_d = dt_in("waug", [17, NL, 1024])
    ggain_d = dt_in("ggain", [128, NL, 2])
    sinks_d = dt_in("sinks", [128, NL, 32])
    cm_d = dt_in("cmask", [128, 4, 128])
    u2_d = dt_in("u2", [128, 2])
    id_d = dt_in("ident", [128, 128])
    yout = nc.dram_tensor("yT", [KB, 128, S], F32, kind="ExternalOutput").ap()
    xs = nc.dram_tensor("xs", [KB, 128, S], F32, kind="Internal").ap()
    groups = weight_groups()
    NG = len(groups)
    gidx = {g[0]: i for i, g in enumerate(groups)}
    scr = [nc.dram_tensor(f"wscr{l}", [NG, 128, 4096], BF16, kind="Internal").ap() for l in range(NL)]

    sch = Sched()
    from contextlib import ExitStack
    with ExitStack() as es:
        def sb(name, shape, dt):
            return es.enter_context(nc.sbuf_tensor(name, shape, dt))

        def ps(name, shape, dt):
            return es.enter_context(nc.psum_tensor(name, shape, dt))

        hT = sb("hT", [128, KB, T], BF16)
        slots = [sb(f"slot{i}", [128, 4096], BF16) for i in range(NSLOT)]
        fs = [sb(f"fs{i}", [128, T], F32) for i in range(6)]
        gdT = sb("gdT", [17, T], F32)
        qtT = sb("qtT", [128, T], BF16)
        ktT = sb("ktT", [128, T], BF16)
        ktm = sb("ktm", [128, 4, 128], BF16)
        vT = sb("vT", [128, 2, T], BF16)
        vtm = sb("vtm", [128, 4, 256], BF16)
        sgT = sb("sgT", [128, 2, T], BF16)
        atb = [sb(f"atb{i}", [128, 128], BF16) for i in range(2)]
        sbf = sb("sbf", [128, 256], BF16)
        onb = sb("onb", [128, 256], BF16)
        junk = sb("junk", [128, 256], BF16)
        Sst = sb("Sst", [128, 8, 256], F32)
        svals = sb("svals", [128, 3, 4], F32)
        ssum = sb("ssum", [128, 2], F32)
        ogT = sb("ogT", [128, 16, T], BF16)
        obT = sb("obT", [128, 16, T], BF16)
        mgT = sb("mgT", [128, KB, T], BF16)
        vs = sb("vs", [128, 5, 8, 68], BF16)
        kT2 = sb("kT2", [128, 8, 640], BF16)
        qTg = [sb(f"qTg{i}", [128, 2, T], BF16) for i in range(2)]
        pc = sb("pc", [128, 4, 128], BF16)
        pp = sb("pp", [128, 4, 128], BF16)
        obb = sb("obb", [128, 4, 64], BF16)
        den = sb("den", [128, 4], F32)
        cmask = sb("cmask_s", [128, 4, 128], F32)
        cmb = sb("cmb", [128, 2, 128], BF16)
        u2 = sb("u2_s", [128, 2], F32)
        identf = sb("identf", [128, 128], F32)
        ident = sb("ident_s", [128, 128], BF16)
        gains = sb("gains_s", [128, NL + 1, KB], F32)
        bg = sb("bg_s", [128, NL, 2, KB], F32)
        waug = sb("waug_s", [17, 1024], F32)
        ggain = sb("ggain_s", [128, NL, 2], F32)
        esink = sb("esink", [128, NL, 32], F32)
        epsc = sb("epsc", [128, 1], F32)
        onec = sb("onec", [128, 1], F32)

        pA = [ps(f"pA{i}", [128, T], F32) for i in range(2)]
        pB = [ps(f"pB{i}", [128, T], F32) for i in range(2)]
        pC = [ps(f"pC{i}", [128, T], F32) for i in range(2)]
        pT = [ps(f"pT{i}", [128, 1024], BF16) for i in range(2)]

        dma_names = [f"slot{i}" for i in range(NSLOT)] + [f"cast{l}" for l in range(NL)] + \
            ["c0", "c1", "c2", "c3", "c4", "c5", "c6", "c7", "c8", "waug"] + [f"fsl{i}" for i in range(6)] + \
            [f"fss{i}" for i in range(6)]
        dma_sems = {n: es.enter_context(nc.semaphore("d_" + n)) for n in dma_names}
        sems = {e: es.enter_context(nc.semaphore("e_" + e)) for e in ["pe", "act", "dve", "pool"]}
        block = es.enter_context(nc.Block())

        def dma(q, out, in_, key, reads=(), writes=(), nodep=False):
            sch.op(q, lambda e, out=out, in_=in_: e.dma_start(out=out, in_=in_), reads=reads, writes=writes,
                   dma=key, nodep=nodep)

        dma("sp", cmask[:], cm_d, "c0", writes=["cmask"])
        dma("sp", u2[:], u2_d, "c1", writes=["u2"])
        dma("sp", identf[:], id_d, "c2", writes=["identf"])
        dma("sp", gains[:], gains_d, "c3", writes=["gains"])
        dma("sp", bg[:], bg_d, "c4", writes=["bg"])
        dma("sp", ggain[:], ggain_d, "c5", writes=["ggain"])
        dma("sp", esink[:], sinks_d, "c6", writes=["esink"])
        sch.op("dve", lambda e: e.tensor_copy(out=ident[:], in_=identf[:]), reads=["identf"], writes=["ident"])
        sch.op("dve", lambda e: e.tensor_copy(out=cmb[:], in_=cmask[:, 0:2, :]), reads=["cmask"], writes=["cmb"])
        sch.op("dve", lambda e: e.memset(epsc[:], EPS), writes=["epsc"])
        sch.op("dve", lambda e: e.memset(onec[:], 1.0), writes=["onec"])
        sch.op("dve", lambda e: e.memset(gdT[:], 1.0), writes=["gdT"])
        for i in range(2):
            sch.op("dve", lambda e, i=i: e.memset(qTg[i][:].rearrange("p a b -> p (a b)"), 0.0), writes=["qTg"])
        sch.op("act", lambda e: e.activation(out=esink[:], in_=esink[:], func=AF.Exp), reads=["esink"],
               writes=["esink"])
        ones_f = cmask[:, 3, :]
        umid = cmask[:, 2, :]

        for l in range(NL):
            for gi, (name, src, cols, nkb) in enumerate(groups):
                ncol = sum(c for _, c in cols)
                dstv = scr[l][gi][:, 0:nkb * ncol].rearrange("p (k c) -> p k c", c=ncol)
                off = 0
                for (c0, c) in cols:
                    srcv = wsrc[src][l][:, c0:c0 + c].rearrange("(k p) c -> p k c", p=128)
                    dma("pool", dstv[:, :, off:off + c], srcv, f"cast{l}", writes=[f"wscr{l}"], nodep=True)
                    off += c

        state = dict(slot_i=0, pa=0, loads=[])

        def fm_group(l, name, rhs_list, out_ps, m=128):
            gi = gidx[name]
            _, _, cols, nkb = groups[gi]
            ncol = sum(c for _, c in cols)
            si = state["slot_i"] % NSLOT
            state["slot_i"] += 1
            slot = slots[si]
            dma("sp", slot[:, 0:nkb * ncol], scr[l][gi][:, 0:nkb * ncol], f"slot{si}", reads=[f"wscr{l}"],
                writes=[f"slot{si}"])
            wv = slot[:, 0:nkb * ncol].rearrange("p (k c) -> p k c", c=ncol)
            rl = list(rhs_list)

            def f(e):
                ins = None
                for k in range(nkb):
                    ins = e.matmul(out_ps[0:m, :], wv[:, k, 0:m], rl[k][0], start=(k == 0), stop=(k == nkb - 1))
                return ins
            rkeys = sorted(set(r[1] for r in rl))
            return f, [f"slot{si}"] + rkeys

        def nextA():
            i = state["pa"] % 2
            state["pa"] += 1
            return pA[i], f"pA{i}"

        hT_rhs = [(hT[:, k, :], f"hT{k}") for k in range(KB)]

        def proj(l, name, evac_eng, evac_fn, ewrites, ereads=(), m=128):
            p, pk = nextA()
            evac_fn = freeze(evac_fn)
            f, r = fm_group(l, name, hT_rhs, p, m=m)
            sch.op("pe", f, reads=r, writes=[pk])
            sch.op(evac_eng, lambda e, p=p: evac_fn(e, p), reads=[pk] + list(ereads), writes=list(ewrites))

        def rmsnorm_tile(l, xsrc, t, final=False):
            tok = slice(t * T, (t + 1) * T)
            pn = pC[1]
            for k in range(KB):
                xb, sq = fs[k % 2], fs[2 + k % 2]
                dma("sp", xb[:], xsrc[k, :, tok], f"fsl{k % 2}", reads=[f"xs{t}_{k}"], writes=[f"fs{k % 2}"])
                sch.op("act", lambda e, xb=xb, sq=sq: e.activation(out=sq[:], in_=xb[:], func=AF.Square),
                       reads=[f"fs{k % 2}"], writes=[f"fs{2 + k % 2}"])
                sch.op("pe", lambda e, sq=sq, k=k: e.matmul(pn[:], ones_f, sq[:], start=(k == 0), stop=(k == KB - 1)),
                       reads=[f"fs{2 + k % 2}", "cmask"], writes=["pC1"])
            rs = fs[4]
            sch.op("act", lambda e: e.activation(out=rs[:], in_=pn[:], func=AF.Ln, bias=epsc[:], scale=1.0 / D),
                   reads=["pC1", "epsc"], writes=["fs4"])
            sch.op("act", lambda e: e.activation(out=rs[:], in_=rs[:], func=AF.Exp, scale=-0.5),
                   reads=["fs4"], writes=["fs4"])
            for k in range(KB):
                xb = fs[k % 2]
                dma("sp", xb[:], xsrc[k, :, tok], f"fsl{k % 2}", reads=[f"xs{t}_{k}"], writes=[f"fs{k % 2}"])
                if not final:
                    sch.op("dve", lambda e, xb=xb, k=k: e.scalar_tensor_tensor(
                        out=hT[:, k, :], in0=xb[:], scalar=gains[:, l, k:k + 1], in1=rs[:], op0=ALU.mult, op1=ALU.mult),
                        reads=[f"fs{k % 2}", "fs4", "gains"], writes=[f"hT{k}"])
                else:
                    ob = fs[2 + k % 2]
                    sch.op("dve", lambda e, xb=xb, k=k, ob=ob: e.scalar_tensor_tensor(
                        out=ob[:], in0=xb[:], scalar=gains[:, l, k:k + 1], in1=rs[:], op0=ALU.mult, op1=ALU.mult),
                        reads=[f"fs{k % 2}", "fs4", "gains"], writes=[f"fs{2 + k % 2}"])
                    dma("act", yout[k, :, tok], ob[:], f"fss{2 + k % 2}", reads=[f"fs{2 + k % 2}"], writes=[f"y{t}_{k}"])

        def transposes(srcs, pt_i, skey, n):
            p = pT[pt_i]

            def f(e):
                ins = None
                for (blk, ap) in srcs:
                    ins = e.transpose(p[:, blk * 128:(blk + 1) * 128], ap, ident[:])
                return ins
            sch.op("pe", f, reads=list(skey) + ["ident"], writes=[f"pT{pt_i}"])

        for l in range(NL if DBG >= 2 else 0):
            xsrc = xin if l == 0 else xs
            dma("sp", waug[:], waug_d[:, l, :], "waug", writes=["waug"])
            for t in range(NT):
                tok = slice(t * T, (t + 1) * T)
                rmsnorm_tile(l, xsrc, t)
                proj(l, "D", "dve", lambda e, p: e.tensor_copy(out=gdT[0:16, :], in_=p[0:16, :]), ["gdT"], m=16)
                if t == 0:
                    sch.op("dve", lambda e: e.memset(Sst[:], 0.0), writes=["Sst"])
                    sch.op("dve", lambda e: e.memset(kT2[:, :, 0:128], 0.0), writes=["kT2"])
                    if l == 0:
                        sch.op("dve", lambda e: e.memset(vs[:].rearrange("p a b c -> p (a b c)"), 0.0), writes=["vs"])
                        for blk in range(1, 5):
                            sch.op("dve", lambda e, blk=blk: e.memset(vs[:, blk, :, 64:65], 1.0), writes=["vs"])
                    else:
                        sch.op("dve", lambda e: e.memset(vs[:, 0, :, :], 0.0), writes=["vs"])
                for h in range(8 if DBG >= 3 else 0):
                    cb_, einv, eE = fs[5], fs[2], fs[3]
                    def f_logit(e, h=h):
                        ins = None
                        for tb in range(4):
                            ins = e.matmul(pB[0][:, tb * 128:(tb + 1) * 128], gdT[:, tb * 128:(tb + 1) * 128],
                                           waug[:, h * 128:(h + 1) * 128], start=True, stop=True)
                        return ins
                    sch.op("pe", f_logit, reads=["gdT", "waug"], writes=["pB0_0", "pB0_1"])
                    sch.op("act", lambda e: e.activation(out=cb_[:], in_=pB[0][:], func=AF.Exp, scale=-1.0),
                           reads=["pB0_0", "pB0_1"], writes=["fs5"])
                    sch.op("act", lambda e: e.activation(out=cb_[:], in_=cb_[:], func=AF.Ln, bias=onec[:], scale=1.0),
                           reads=["fs5", "onec"], writes=["fs5"])
                    sch.op("dve", lambda e: e.tensor_scalar(out=cb_[:], in0=cb_[:], scalar1=1.0 / 16.0, scalar2=1.0,
                                                            op0=ALU.mult, op1=ALU.min), reads=["fs5"], writes=["fs5"])
                    def f_cum(e):
                        ins = None
                        for tb in range(4):
                            e.matmul(pB[1][:, tb * 128:(tb + 1) * 128], cb_[:, tb * 128:(tb + 1) * 128], umid,
                                     start=True, stop=True)
                            ins = e.matmul(pC[1][:, tb * 2:tb * 2 + 2], cb_[:, tb * 128:(tb + 1) * 128], u2[:],
                                           start=True, stop=True)
                        return ins
                    sch.op("pe", f_cum, reads=["fs5", "cmask", "u2"], writes=["pB1_0", "pB1_1", "pC1"])
                    sch.op("act", lambda e: e.activation(out=einv[:], in_=pB[1][:], func=AF.Exp, scale=-1.0),
                           reads=["pB1_0", "pB1_1"], writes=["fs2"])
                    sch.op("act", lambda e: e.activation(out=eE[:], in_=pB[1][:], func=AF.Exp, scale=1.0),
                           reads=["pB1_0", "pB1_1"], writes=["fs3"])
                    bref = pC[1][:, 0:8].rearrange("p (t two) -> p t two", two=2)
                    sch.op("act", lambda e: e.activation(out=svals[:, 0, :], in_=bref[:, :, 0], func=AF.Exp, scale=-1.0),
                           reads=["pC1"], writes=["svals"])
                    sch.op("act", lambda e: e.activation(out=svals[:, 1, :], in_=bref[:, :, 0], func=AF.Exp, scale=1.0),
                           reads=["pC1"], writes=["svals"])
                    sch.op("act", lambda e: e.activation(out=svals[:, 2, :], in_=bref[:, :, 1], func=AF.Exp, scale=-1.0),
                           reads=["pC1"], writes=["svals"])
                    proj(l, f"Q{h}", "dve", lambda e, p: e.scalar_tensor_tensor(
                        out=qtT[:], in0=p[:], scalar=128 ** -0.5, in1=einv[:], op0=ALU.mult, op1=ALU.mult),
                        ["qtT"], ["fs2"])
                    proj(l, f"K{h}", "dve", lambda e, p: e.tensor_tensor(out=ktT[:], in0=p[:], in1=eE[:], op=ALU.mult),
                         ["ktT"], ["fs3"])
                    transposes([(tb, ktT[:, tb * 128:(tb + 1) * 128]) for tb in range(4)], 0, ["ktT"], 4)
                    sch.op("act", lambda e: e.copy(out=ktm[:].rearrange("p a b -> p (a b)"), in_=pT[0][:, 0:512]),
                           reads=["pT0"], writes=["ktm"])
                    for vb in range(2):
                        proj(l, f"V{h}_{vb}", "act", lambda e, p, vb=vb: e.copy(out=vT[:, vb, :], in_=p[:]), ["vT"])
                    transposes([(tb * 2 + vb, vT[:, vb, tb * 128:(tb + 1) * 128]) for tb in range(4) for vb in range(2)],
                               1, ["vT"], 8)
                    sch.op("dve", lambda e: e.tensor_copy(out=vtm[:].rearrange("p a b -> p (a b)"), in_=pT[1][:]),
                           reads=["pT1"], writes=["vtm"])
                    for vb in range(2):
                        proj(l, f"G{h}_{vb}", "act", lambda e, p, vb=vb: e.activation(out=sgT[:, vb, :], in_=p[:],
                                                                                      func=AF.Silu), ["sgT"])
                    for tb in range(4):
                        tsl = slice(tb * 128, (tb + 1) * 128)
                        ab = atb[tb % 2]
                        pat = pB[0][:, (tb % 2) * 128:(tb % 2) * 128 + 128]
                        po = pC[0][:, (tb % 2) * 256:(tb % 2) * 256 + 256]
                        pP = pB[1][:, (tb % 2) * 256:(tb % 2) * 256 + 256]
                        sch.op("pe", lambda e, pat=pat, tsl=tsl: e.matmul(pat, ktT[:, tsl], qtT[:, tsl], start=True, stop=True),
                               reads=["ktT", "qtT"], writes=[f"pB0_{tb % 2}"])
                        sch.op("dve", lambda e, pat=pat, ab=ab: e.tensor_tensor(out=ab[:], in0=pat, in1=cmask[:, 0, :],
                                                                                 op=ALU.mult),
                               reads=[f"pB0_{tb % 2}", "cmask"], writes=[f"atb{tb % 2}"])
                        sch.op("dve", lambda e, h=h, tb=tb: e.tensor_scalar(out=sbf[:], in0=Sst[:, h, :],
                                                                            scalar1=svals[:, 0, tb:tb + 1], scalar2=None,
                                                                            op0=ALU.mult),
                               reads=["Sst", "svals"], writes=["sbf"])

                        def f_o(e, po=po, ab=ab, tb=tb, tsl=tsl):
                            e.matmul(po, ab[:], vtm[:, tb, :], start=True, stop=False)
                            return e.matmul(po, qtT[:, tsl], sbf[:], start=False, stop=True)
                        sch.op("pe", f_o, reads=[f"atb{tb % 2}", "vtm", "qtT", "sbf"], writes=[f"pC0_{tb % 2}"])
                        sch.op("pe", lambda e, pP=pP, tb=tb: e.matmul(pP, ktm[:, tb, :], vtm[:, tb, :], start=True, stop=True),
                               reads=["ktm", "vtm"], writes=[f"pB1_{tb % 2}"])
                        sch.op("dve", lambda e, pP=pP, h=h, tb=tb: e.scalar_tensor_tensor(
                            out=Sst[:, h, :], in0=pP, scalar=svals[:, 1, tb:tb + 1], in1=Sst[:, h, :],
                            op0=ALU.mult, op1=ALU.add), reads=[f"pB1_{tb % 2}", "svals", "Sst"], writes=["Sst"])
                        sch.op("dve", lambda e, h=h, tb=tb: e.tensor_scalar(
                            out=Sst[:, h, :], in0=Sst[:, h, :], scalar1=svals[:, 2, tb:tb + 1], scalar2=None, op0=ALU.mult),
                            reads=["Sst", "svals"], writes=["Sst"])
                        sch.op("act", lambda e, po=po: e.activation(out=junk[:], in_=po, func=AF.Square,
                                                                    accum_out=ssum[:, 0:1]),
                               reads=[f"pC0_{tb % 2}"], writes=["junk", "ssum"])
                        sch.op("act", lambda e: e.activation(out=ssum[:, 1:2], in_=ssum[:, 0:1], func=AF.Ln, bias=epsc[:],
                                                             scale=1.0 / 256.0), reads=["ssum", "epsc"], writes=["ssum"])
                        sch.op("act", lambda e: e.activation(out=ssum[:, 1:2], in_=ssum[:, 1:2], func=AF.Exp, scale=-0.5),
                               reads=["ssum"], writes=["ssum"])
                        sch.op("dve", lambda e, po=po: e.tensor_scalar(out=onb[:], in0=po, scalar1=ssum[:, 1:2],
                                                                       scalar2=None, op0=ALU.mult),
                               reads=[f"pC0_{tb % 2}", "ssum"], writes=["onb"])
                        transposes([(vb, onb[:, vb * 128:(vb + 1) * 128]) for vb in range(2)], 0, ["onb"], 2)
                        for vb in range(2):
                            sch.op("dve", lambda e, vb=vb, h=h, tsl=tsl: e.scalar_tensor_tensor(
                                out=ogT[:, 2 * h + vb, tsl], in0=pT[0][:, vb * 128:(vb + 1) * 128],
                                scalar=ggain[:, l, vb:vb + 1], in1=sgT[:, vb, tsl], op0=ALU.mult, op1=ALU.mult),
                                reads=["pT0", "ggain", "sgT"], writes=["ogT"])
                for cb in range(4 if DBG >= 4 else 0):
                    proj(l, f"VS{cb}", "act", lambda e, p: e.copy(out=vT[:, 0, :], in_=p[:]), ["vT"])
                    transposes([(tb, vT[:, 0, tb * 128:(tb + 1) * 128]) for tb in range(4)], 1, ["vT"], 4)
                    for tb in range(4):
                        sch.op("dve", lambda e, cb=cb, tb=tb: e.tensor_copy(
                            out=vs[:, 1 + tb, 2 * cb:2 * cb + 2, 0:64],
                            in_=pT[1][:, tb * 128:(tb + 1) * 128].rearrange("p (g d) -> p g d", g=2)),
                            reads=["pT1"], writes=["vs"])
                for s in range(8 if DBG >= 4 else 0):
                    proj(l, f"KK{s}", "act", lambda e, p, s=s: e.copy(out=kT2[:, s, 128:640], in_=p[:]), ["kT2"])
                    for pr in range(2):
                        def ev_q(e, p, pr=pr):
                            e.tensor_scalar(out=qTg[0][0:64, pr, :], in0=p[0:64, :], scalar1=0.125, scalar2=None,
                                            op0=ALU.mult)
                            return e.tensor_scalar(out=qTg[1][64:128, pr, :], in0=p[64:128, :], scalar1=0.125,
                                                   scalar2=None, op0=ALU.mult)
                        proj(l, f"SQ{s}_{pr}", "dve", ev_q, ["qTg"])
                    for pr in range(2):
                        proj(l, f"SG{s}_{pr}", "act", lambda e, p, pr=pr: e.activation(out=sgT[:, pr, :], in_=p[:],
                                                                                       func=AF.Silu), ["sgT"])
                    for qb in range(4 if SUB >= 2 else 0):
                        qsl = slice(qb * 128, (qb + 1) * 128)

                        def f_sc(e, s=s, qb=qb, qsl=qsl):
                            ins = None
                            for r in range(4):
                                e.matmul(pB[0][:, r * 128:(r + 1) * 128], kT2[:, s, (qb + 1) * 128:(qb + 2) * 128],
                                         qTg[r % 2][:, r // 2, qsl], start=True, stop=True)
                                ins = e.matmul(pB[1][:, r * 128:(r + 1) * 128], kT2[:, s, qb * 128:(qb + 1) * 128],
                                               qTg[r % 2][:, r // 2, qsl], start=True, stop=True)
                            return ins
                        sch.op("pe", f_sc, reads=["kT2", "qTg"], writes=["pB0_0", "pB0_1", "pB1_0", "pB1_1"])
                        sch.op("act", lambda e: e.activation(out=pc[:].rearrange("p a b -> p (a b)"), in_=pB[0][:],
                                                             func=AF.Exp), reads=["pB0_0", "pB0_1"], writes=["pc"])
                        sch.op("act", lambda e: e.activation(out=pp[:].rearrange("p a b -> p (a b)"), in_=pB[1][:],
                                                             func=AF.Exp), reads=["pB1_0", "pB1_1"], writes=["pp"])
                        if SUB < 3:
                            continue
                        for r in range(4):
                            sch.op("dve", lambda e, r=r: e.tensor_tensor(out=pc[:, r, :], in0=pc[:, r, :], in1=cmb[:, 0, :],
                                                                         op=ALU.mult), reads=["pc", "cmb"], writes=["pc"])
                            sch.op("dve", lambda e, r=r: e.tensor_tensor(out=pp[:, r, :], in0=pp[:, r, :], in1=cmb[:, 1, :],
                                                                         op=ALU.mult), reads=["pp", "cmb"], writes=["pp"])

                        def f_pv(e, s=s, qb=qb):
                            ins = None
                            for r in range(4):
                                e.matmul(pC[0][:, r * 128:r * 128 + 68], pc[:, r, :], vs[:, qb + 1, s, :], start=True, stop=False)
                                ins = e.matmul(pC[0][:, r * 128:r * 128 + 68], pp[:, r, :], vs[:, qb, s, :], start=False, stop=True)
                            return ins
                        sch.op("pe", f_pv, reads=["pc", "pp", "vs"], writes=["pC0_0", "pC0_1"])
                        if SUB < 4:
                            continue
                        pov = pC[0][:].rearrange("p (r d) -> p r d", d=128)
                        sch.op("dve", lambda e, s=s, pov=pov: e.tensor_tensor(out=den[:], in0=pov[:, :, 64],
                                                                              in1=esink[:, l, 4 * s:4 * s + 4], op=ALU.add),
                               reads=["pC0_0", "pC0_1", "esink"], writes=["den"])
                        sch.op("dve", lambda e: e.reciprocal(out=den[:], in_=den[:]), reads=["den"], writes=["den"])
                        for r in range(4):
                            sch.op("dve", lambda e, r=r, pov=pov: e.tensor_scalar(
                                out=obb[:, r, :], in0=pov[:, r, 0:64], scalar1=den[:, r:r + 1], scalar2=None, op0=ALU.mult),
                                reads=["pC0_0", "pC0_1", "den"], writes=["obb"])
                        obf = obb[:].rearrange("p a b -> p (a b)")
                        transposes([(pr, obf[:, pr * 128:(pr + 1) * 128]) for pr in range(2)], 0, ["obb"], 2)
                        for pr in range(2):
                            sch.op("dve", lambda e, pr=pr, s=s, qsl=qsl: e.tensor_tensor(
                                out=obT[:, 2 * s + pr, qsl], in0=pT[0][:, pr * 128:(pr + 1) * 128], in1=sgT[:, pr, qsl],
                                op=ALU.mult), reads=["pT0", "sgT"], writes=["obT"])
                sch.op("dve", lambda e: e.tensor_copy(out=kT2[:, :, 0:128], in_=kT2[:, :, 512:640]), reads=["kT2"],
                       writes=["kT2"])
                sch.op("dve", lambda e: e.tensor_copy(out=vs[:, 0, :, :], in_=vs[:, 4, :, :]), reads=["vs"], writes=["vs"])
                og_rhs = [(ogT[:, k, :], "ogT") for k in range(16)]
                ob_rhs = [(obT[:, k, :], "obT") for k in range(16)]
                for n in range(32 if DBG >= 5 else 0):
                    f, r = fm_group(l, f"GA{n}", hT_rhs, pA[0])
                    sch.op("pe", f, reads=r, writes=["pA0"])
                    f, r = fm_group(l, f"GB{n}", hT_rhs, pA[1])
                    sch.op("pe", f, reads=r, writes=["pA1"])
                    f, r = fm_group(l, f"WA{n}", og_rhs, pB[0])
                    sch.op("pe", f, reads=r, writes=["pB0_0", "pB0_1"])
                    f, r = fm_group(l, f"WB{n}", ob_rhs, pB[1])
                    sch.op("pe", f, reads=r, writes=["pB1_0", "pB1_1"])
                    sa, sbb, m1 = fs[2], fs[3], fs[5]
                    sch.op("act", lambda e, n=n: e.activation(out=sa[:], in_=pA[0][:], func=AF.Sigmoid,
                                                              bias=bg[:, l, 0, n:n + 1], scale=1.0),
                           reads=["pA0", "bg"], writes=["fs2"])
                    sch.op("act", lambda e, n=n: e.activation(out=sbb[:], in_=pA[1][:], func=AF.Sigmoid,
                                                              bias=bg[:, l, 1, n:n + 1], scale=1.0),
                           reads=["pA1", "bg"], writes=["fs3"])
                    sch.op("dve", lambda e: e.tensor_tensor(out=m1[:], in0=pB[0][:], in1=sa[:], op=ALU.mult),
                           reads=["pB0_0", "pB0_1", "fs2"], writes=["fs5"])
                    sch.op("dve", lambda e: e.tensor_tensor(out=sbb[:], in0=pB[1][:], in1=sbb[:], op=ALU.mult),
                           reads=["pB1_0", "pB1_1", "fs3"], writes=["fs3"])
                    sch.op("dve", lambda e, n=n: e.tensor_tensor(out=mgT[:, n, :], in0=m1[:], in1=sbb[:], op=ALU.add),
                           reads=["fs5", "fs3"], writes=["mgT"])
                mg_rhs = [(mgT[:, k, :], "mgT") for k in range(KB)]
                for m in range(32 if DBG >= 6 else 0):
                    p, pk = nextA()
                    f, r = fm_group(l, f"WO{m}", mg_rhs, p)
                    sch.op("pe", f, reads=r, writes=[pk])
                    xb = fs[m % 2]
                    dma("sp", xb[:], xsrc[m, :, tok], f"fsl{m % 2}", reads=[f"xs{t}_{m}"], writes=[f"fs{m % 2}"])
                    sch.op("dve", lambda e, p=p, xb=xb: e.tensor_tensor(out=xb[:], in0=p[:], in1=xb[:], op=ALU.add),
                           reads=[pk, f"fs{m % 2}"], writes=[f"fs{m % 2}"])
                    dma("act", xs[m, :, tok], xb[:], f"fss{m % 2}", reads=[f"fs{m % 2}"], writes=[f"xs{t}_{m}"])
        for t in range(NT):
            rmsnorm_tile(NL, xs if DBG >= 6 else xin, t, final=True)
        sch.op("sp", None, reads=[f"y{t}_{k}" for t in range(NT) for k in range(KB)])
        sch.emit(nc, block, sems, dma_sems)
    return nc


def _consts():
    j = np.arange(128)[:, None]
    i = np.arange(128)[None, :]
    cm = np.zeros((128, 4, 128), np.float32)
    cm[:, 0, :] = (j <= i)
    cm[:, 1, :] = (j > i)
    cm[:, 2, :] = (j <= i).astype(np.float32) - (j <= 64).astype(np.float32)
    cm[:, 3, :] = 1.0
    u2 = np.zeros((128, 2), np.float32)
    u2[:, 0] = (np.arange(128) <= 64)
    u2[:, 1] = 1.0
    return cm, u2, np.eye(128, dtype=np.float32)


def _layout_small(NL, norm_gains, b_gates, w_decay_up, b_decay, gla_norm_gains, sinks, final_norm_gain):
    g = np.concatenate([norm_gains[:NL], final_norm_gain[None]], 0)
    gains = np.ascontiguousarray(g.reshape(NL + 1, KB, 128).transpose(2, 0, 1))
    bgates = np.ascontiguousarray(b_gates[:NL].reshape(NL, 2, KB, 128).transpose(3, 0, 1, 2))
    waug = np.ascontiguousarray(np.concatenate([w_decay_up[:NL], b_decay[:NL, None, :]], 1).transpose(1, 0, 2))
    ggain = np.ascontiguousarray(gla_norm_gains[:NL].reshape(NL, 2, 128).transpose(2, 0, 1))
    sk = np.ascontiguousarray(np.broadcast_to(sinks[:NL][None], (128, NL, 32)))
    return dict(gains=gains, bgates=bgates, waug=waug, ggain=ggain, sinks=sk)


def run_cores(xb_list, NL, NT, weights, small, trace=False):
    nc = build_nc(NL, NT)
    cm, u2, ident = _consts()
    in_maps = []
    for xb in xb_list:
        xT = np.ascontiguousarray(xb.T.reshape(KB, 128, NT * T))
        m = dict(xT=xT, cmask=cm, u2=u2, ident=ident)
        m.update(weights)
        m.update(small)
        in_maps.append(m)
    res = run_bass_kernel_spmd(nc, in_maps, core_ids=list(range(len(xb_list))), trace=trace)
    outs = [np.ascontiguousarray(r["yT"].reshape(D, NT * T).T) for r in res.results]
    return outs, res


def kernel(x, norm_gains, w_in, b_gates, w_decay_up, b_decay, gla_norm_gains, sinks, w_gla_out, w_swa_out,
           w_out, final_norm_gain):
    x = np.asarray(x)
    B, S, _ = x.shape
    NL = w_in.shape[0]
    weights = dict(w_in=np.asarray(w_in), w_gla_out=np.asarray(w_gla_out), w_swa_out=np.asarray(w_swa_out),
                   w_out=np.asarray(w_out))
    small = _layout_small(NL, np.asarray(norm_gains), np.asarray(b_gates), np.asarray(w_decay_up),
                          np.asarray(b_decay), np.asarray(gla_norm_gains), np.asarray(sinks),
                          np.asarray(final_norm_gain))
    outs, _ = run_cores([x[b] for b in range(B)], NL, S // T, weights, small)
    return np.stack(outs, 0).astype(np.float32)
```
